# Optimizing a Trainium2 kernel written in Bass

```python
import math
import jax, jax.numpy as jnp
from jax import lax
import numpy as np

D_MODEL = 1024
BATCH = 4
SEQ = 4096
DEPTH = 2

HEAD_DIM = 64
N_EVEN = (DEPTH + 1) // 2
N_ODD = DEPTH // 2
A_HEADS = D_MODEL // (4 * HEAD_DIM)
B_HEADS = D_MODEL // (2 * HEAD_DIM)
IDX_HEADS = 8
IDX_DIM = 64
TOPK_MAX = 256
C_HEADS = D_MODEL // HEAD_DIM
C_KV_HEADS = C_HEADS // 4
WINDOW = 128
FFN_HIDDEN = ((8 * D_MODEL // 3 + 255) // 256) * 256
Q_BLOCK = 128
NORM_EPS = 1e-6

EVEN_SIZES = (A_HEADS * 2 * HEAD_DIM,
              A_HEADS * 2 * HEAD_DIM,
              A_HEADS * 2 * HEAD_DIM,
              B_HEADS * HEAD_DIM,
              HEAD_DIM,
              HEAD_DIM,
              IDX_HEADS * IDX_DIM,
              IDX_DIM,
              IDX_HEADS)
EVEN_COLS = sum(EVEN_SIZES)
ODD_SIZES = (C_HEADS * HEAD_DIM, C_KV_HEADS * HEAD_DIM, C_KV_HEADS * HEAD_DIM)
ODD_COLS = sum(ODD_SIZES)

kernel_name = 'hybrid_diffattn_dsa_swa_sink_adaln'


def _rms_norm(x, g):
    xf = x.astype(jnp.float32)
    y = xf * lax.rsqrt(jnp.mean(xf * xf, axis=-1, keepdims=True) + NORM_EPS)
    return (y * g.astype(jnp.float32)).astype(x.dtype)


def _split_cols(t, sizes):
    offs = np.cumsum(np.asarray(sizes))[:-1].tolist()
    return jnp.split(t, offs, axis=-1)


def _alibi_slopes(n):
    return jnp.asarray(2.0 ** (-8.0 * np.arange(1, n + 1) / n), dtype=jnp.float32)


def _diff_attention(q, k, v, lam, slopes):
    b, s, h = q.shape[:3]
    nb = s // Q_BLOCK
    qb = jnp.moveaxis(q.reshape(b, nb, Q_BLOCK, h, 2, HEAD_DIM), 1, 0)
    key_pos = jnp.arange(s)
    scale = HEAD_DIM ** -0.5

    def block(args):
        qi, i = args
        dist = (i * Q_BLOCK + jnp.arange(Q_BLOCK))[:, None] - key_pos[None, :]
        logits = jnp.einsum('bqhmd,bshmd->bhmqs', qi, k).astype(jnp.float32) * scale
        logits = logits - slopes[None, :, None, None, None] * dist.astype(jnp.float32)
        logits = jnp.where(dist >= 0, logits, -jnp.inf)
        p = jax.nn.softmax(logits, axis=-1)
        a = p[:, :, 0] - lam * p[:, :, 1]
        return jnp.einsum('bhqs,bshe->bqhe', a.astype(v.dtype), v)

    out = lax.map(block, (qb, jnp.arange(nb)))
    return jnp.moveaxis(out, 0, 1).reshape(b, s, h, 2 * HEAD_DIM)


def _dsa_attention(q, k, v, iq, ik, iw, slopes, topk):
    b, s, h = q.shape[:3]
    nb = s // Q_BLOCK
    key_pos = jnp.arange(s)
    scale = HEAD_DIM ** -0.5
    gather = jax.vmap(lambda t, idx: t[idx])

    def blocks(t):
        return jnp.moveaxis(t.reshape((b, nb, Q_BLOCK) + t.shape[2:]), 1, 0)

    def block(args):
        qi, iqi, iwi, i = args
        q_pos = i * Q_BLOCK + jnp.arange(Q_BLOCK)
        causal = key_pos[None, :] <= q_pos[:, None]
        idx_logits = jnp.einsum('bqhd,bsd->bqhs', iqi, ik).astype(jnp.float32) * IDX_DIM ** -0.5
        score = jnp.einsum('bqhs,bqh->bqs', jax.nn.relu(idx_logits), iwi.astype(jnp.float32))
        score = jnp.where(causal[None], score, -jnp.inf)
        _, sel = lax.top_k(score, topk)
        k_sel = gather(k, sel)
        v_sel = gather(v, sel)
        dist = (q_pos[None, :, None] - sel)[:, None]
        logits = jnp.einsum('bqhd,bqkd->bhqk', qi, k_sel).astype(jnp.float32) * scale
        logits = logits - slopes[None, :, None, None] * dist.astype(jnp.float32)
        logits = jnp.where(dist >= 0, logits, -jnp.inf)
        p = jax.nn.softmax(logits, axis=-1)
        return jnp.einsum('bhqk,bqkd->bqhd', p.astype(v.dtype), v_sel)

    out = lax.map(block, (blocks(q), blocks(iq), blocks(iw), jnp.arange(nb)))
    return jnp.moveaxis(out, 0, 1).reshape(b, s, h, HEAD_DIM)


def _swa_sink_attention(q, k, v, sinks, slopes):
    b, s = q.shape[:2]
    nb = s // WINDOW
    g = C_HEADS // C_KV_HEADS
    qb = q.reshape(b, nb, WINDOW, C_KV_HEADS, g, HEAD_DIM)

    def band(t):
        tb = t.reshape(b, nb, WINDOW, C_KV_HEADS, HEAD_DIM)
        prev = jnp.pad(tb, ((0, 0), (1, 0), (0, 0), (0, 0), (0, 0)))[:, :-1]
        return jnp.concatenate([prev, tb], axis=2)

    kk, vv = band(k), band(v)
    i = jnp.arange(WINDOW)
    j = jnp.arange(2 * WINDOW)
    dist = WINDOW + i[:, None] - j[None, :]
    key_pos = jnp.arange(nb)[:, None] * WINDOW - WINDOW + j[None, :]
    valid = ((dist >= 0) & (dist < WINDOW))[None] & (key_pos >= 0)[:, None, :]
    logits = jnp.einsum('bnqhgd,bnkhd->bnhgqk', qb, kk).astype(jnp.float32) * HEAD_DIM ** -0.5
    sl = slopes.reshape(C_KV_HEADS, g)
    logits = logits - sl[None, None, :, :, None, None] * dist.astype(jnp.float32)
    logits = jnp.where(valid[None, :, None, None], logits, -jnp.inf)
    sink = jnp.broadcast_to(sinks.astype(jnp.float32).reshape(1, 1, C_KV_HEADS, g, 1, 1),
                            logits.shape[:-1] + (1,))
    p = jax.nn.softmax(jnp.concatenate([logits, sink], axis=-1), axis=-1)[..., :-1]
    out = jnp.einsum('bnhgqk,bnkhd->bnqhgd', p.astype(v.dtype), vv)
    return out.reshape(b, s, C_HEADS * HEAD_DIM)


def _even_mixer(h, w_in, qn_a, kn_a, lam_q1, lam_k1, lam_q2, lam_k2, subln_a, qn_b, kn_b, layer_idx):
    b, s, _ = h.shape
    aq, ak, av, bq, bk, bv, iq, ik, iw = _split_cols(h @ w_in, EVEN_SIZES)
    aq = _rms_norm(aq.reshape(b, s, A_HEADS, 2, HEAD_DIM), qn_a)
    ak = _rms_norm(ak.reshape(b, s, A_HEADS, 2, HEAD_DIM), kn_a)
    av = av.reshape(b, s, A_HEADS, 2 * HEAD_DIM)
    lam_init = 0.8 - 0.6 * math.exp(-0.3 * layer_idx)
    f32 = jnp.float32
    lam = (jnp.exp(jnp.sum(lam_q1.astype(f32) * lam_k1.astype(f32)))
           - jnp.exp(jnp.sum(lam_q2.astype(f32) * lam_k2.astype(f32))) + lam_init)
    ya = _diff_attention(aq, ak, av, lam, _alibi_slopes(A_HEADS))
    ya = _rms_norm(ya, subln_a) * (1.0 - lam_init)
    bq = _rms_norm(bq.reshape(b, s, B_HEADS, HEAD_DIM), qn_b)
    bk = _rms_norm(bk, kn_b)
    iq = iq.reshape(b, s, IDX_HEADS, IDX_DIM)
    iw = iw * IDX_HEADS ** -0.5
    topk = min(TOPK_MAX, s // 4)
    yb = _dsa_attention(bq, bk, bv, iq, ik, iw, _alibi_slopes(B_HEADS), topk)
    return jnp.concatenate([ya.reshape(b, s, -1), yb.reshape(b, s, -1)], axis=-1)


def _odd_mixer(h, w_in, qn_c, kn_c, sinks):
    b, s, _ = h.shape
    q, k, v = _split_cols(h @ w_in, ODD_SIZES)
    q = _rms_norm(q.reshape(b, s, C_HEADS, HEAD_DIM), qn_c)
    k = _rms_norm(k.reshape(b, s, C_KV_HEADS, HEAD_DIM), kn_c)
    v = v.reshape(b, s, C_KV_HEADS, HEAD_DIM)
    return _swa_sink_attention(q, k, v, sinks, _alibi_slopes(C_HEADS))


def _swiglu(h, wg, wu, wd):
    return (jax.nn.silu(h @ wg) * (h @ wu)) @ wd


def setup_inputs(seed: int = 0) -> dict:
    key = jax.random.key(seed)
    ks = iter(list(jax.random.split(key, 32)))
    d, f = D_MODEL, FFN_HIDDEN

    def nrm(shape, scale):
        return jax.random.normal(next(ks), shape, jnp.float32) * scale

    def gain(shape):
        return 1.0 + nrm(shape, 0.02)

    return {
        'x': nrm((BATCH, SEQ, d), 1.0),
        'c': nrm((BATCH, d), 1.0),
        'ada_w': nrm((DEPTH, d, 6 * d), 0.5 * d ** -0.5),
        'ada_b': nrm((DEPTH, 6 * d), 0.02),
        'norm_mix': gain((DEPTH, d)),
        'norm_ffn': gain((DEPTH, d)),
        'w_out': nrm((DEPTH, d, d), d ** -0.5),
        'ffn_gate': nrm((DEPTH, d, f), d ** -0.5),
        'ffn_up': nrm((DEPTH, d, f), d ** -0.5),
        'ffn_down': nrm((DEPTH, f, d), f ** -0.5),
        'w_in_even': nrm((N_EVEN, d, EVEN_COLS), d ** -0.5),
        'qn_a': gain((N_EVEN, HEAD_DIM)),
        'kn_a': gain((N_EVEN, HEAD_DIM)),
        'lam_q1': nrm((N_EVEN, HEAD_DIM), 0.1),
        'lam_k1': nrm((N_EVEN, HEAD_DIM), 0.1),
        'lam_q2': nrm((N_EVEN, HEAD_DIM), 0.1),
        'lam_k2': nrm((N_EVEN, HEAD_DIM), 0.1),
        'subln_a': gain((N_EVEN, 2 * HEAD_DIM)),
        'qn_b': gain((N_EVEN, HEAD_DIM)),
        'kn_b': gain((N_EVEN, HEAD_DIM)),
        'w_in_odd': nrm((N_ODD, d, ODD_COLS), d ** -0.5),
        'qn_c': gain((N_ODD, HEAD_DIM)),
        'kn_c': gain((N_ODD, HEAD_DIM)),
        'sinks_c': nrm((N_ODD, C_HEADS), 0.5),
    }


def reference(x, c, ada_w, ada_b, norm_mix, norm_ffn, w_out, ffn_gate, ffn_up, ffn_down,
              w_in_even, qn_a, kn_a, lam_q1, lam_k1, lam_q2, lam_k2, subln_a, qn_b, kn_b,
              w_in_odd, qn_c, kn_c, sinks_c):
    cond = jax.nn.silu(c)
    for l in range(DEPTH):
        mod = cond @ ada_w[l] + ada_b[l]
        sh1, sc1, g1, sh2, sc2, g2 = [m[:, None, :] for m in jnp.split(mod, 6, axis=-1)]
        h = _rms_norm(x, norm_mix[l]) * (1.0 + sc1) + sh1
        if l % 2 == 0:
            e = l // 2
            y = _even_mixer(h, w_in_even[e], qn_a[e], kn_a[e], lam_q1[e], lam_k1[e],
                            lam_q2[e], lam_k2[e], subln_a[e], qn_b[e], kn_b[e], l)
        else:
            o = l // 2
            y = _odd_mixer(h, w_in_odd[o], qn_c[o], kn_c[o], sinks_c[o])
        x = x + g1 * (y @ w_out[l])
        h = _rms_norm(x, norm_ffn[l]) * (1.0 + sc2) + sh2
        x = x + g2 * _swiglu(h, ffn_gate[l], ffn_up[l], ffn_down[l])
    return x
```

```python
import contextlib
import numpy as np
import concourse.bass as bass
import concourse.mybir as mybir
from concourse.bass_utils import run_bass_kernel_spmd

F32 = mybir.dt.float32
BF16 = mybir.dt.bfloat16
ALU = mybir.AluOpType
AF = mybir.ActivationFunctionType
AX = mybir.AxisListType

EPOCH = 4096
NDMA = 24


class T:
    __slots__ = ("t", "w", "r")

    def __init__(self, t):
        self.t = t
        self.w = []
        self.r = {}

    def __getitem__(self, idx):
        return self.t[idx]


class KB:
    def __init__(self, nc, stack):
        self.nc = nc
        self.stack = stack
        self.engs = {"pe": nc.tensor, "act": nc.scalar, "dve": nc.vector,
                     "pool": nc.gpsimd, "sp": nc.sync}
        self.cnt = {e: 0 for e in self.engs}
        self.sems = {e: [] for e in self.engs}
        self.seen = {e: {} for e in self.engs}
        self.dsem = [stack.enter_context(nc.semaphore("dma%d" % i)) for i in range(NDMA)]
        self.dtot = [0] * NDMA
        self.dnext = 0
        self.nwait = 0

    def sb(self, name, shape, dt):
        return T(self.stack.enter_context(self.nc.sbuf_tensor("s_" + name, list(shape), dt)))

    def ps(self, name, shape, dt):
        return T(self.stack.enter_context(self.nc.psum_tensor("p_" + name, list(shape), dt)))

    def _sem(self, e, epoch):
        while len(self.sems[e]) <= epoch:
            self.sems[e].append(self.stack.enter_context(
                self.nc.semaphore("s_%s_%d" % (e, len(self.sems[e])))))
        return self.sems[e][epoch]

    def _wait(self, e, tok):
        h = self.engs[e]
        if tok[0] == "dma":
            _, slot, val = tok
            key = ("dma", slot)
            if self.seen[e].get(key, 0) >= val:
                return
            h.wait_ge(self.dsem[slot], val)
            self.seen[e][key] = val
        else:
            f, i = tok
            if f == e and e == "pe":
                return
            ep, v = (i - 1) // EPOCH, (i - 1) % EPOCH + 1
            key = (f, ep)
            if self.seen[e].get(key, 0) >= v:
                return
            for (kf, kep), kv in list(self.seen[e].items()):
                if kf == f and isinstance(kep, int) and kep > ep:
                    return
            h.wait_ge(self._sem(f, ep), v)
            self.seen[e][key] = v
        self.nwait += 1

    def _deps(self, e, reads, writes):
        for t in reads:
            for tok in t.w:
                self._wait(e, tok)
        for t in writes:
            for tok in t.w:
                self._wait(e, tok)
            for tok in t.r.values():
                self._wait(e, tok)

    def _mark(self, tok, reads, writes):
        for t in writes:
            t.w = [tok]
            t.r = {}
        for t in reads:
            if t in writes:
                continue
            if tok[0] == "dma":
                t.r[("dma", tok[1])] = tok
            else:
                t.r[tok[0]] = tok

    def op(self, e, fn, reads=(), writes=()):
        self._deps(e, reads, writes)
        ins = fn(self.engs[e])
        self.cnt[e] += 1
        i = self.cnt[e]
        ins.then_inc(self._sem(e, (i - 1) // EPOCH), 1)
        tok = (e, i)
        self._mark(tok, reads, writes)
        return tok

    def dma(self, q, out, in_, reads=(), writes=(), **kw):
        slot = self.dnext
        self.dnext = (self.dnext + 1) % NDMA
        if self.dtot[slot]:
            self._wait(q, ("dma", slot, self.dtot[slot]))
        self._deps(q, reads, writes)
        self.dtot[slot] += 16
        self.engs[q].dma_start(out=out, in_=in_, **kw).then_inc(self.dsem[slot], 16)
        tok = ("dma", slot, self.dtot[slot])
        self._mark(tok, reads, writes)
        return tok

    def barrier_tokens(self):
        toks = [(e, self.cnt[e]) for e in self.engs if self.cnt[e]]
        toks += [("dma", s, self.dtot[s]) for s in range(NDMA) if self.dtot[s]]
        return toks

    def wait_all(self, e):
        for tok in self.barrier_tokens():
            self._wait(e, tok)


D = 1024
NBLK = 32
NOWN = 16
FH = 2816
NHC = 22
EPS = 1e-6


def _dram(nc, name, shape, dt=F32, kind="ExternalInput"):
    return nc.dram_tensor(name, list(shape), dt, kind=kind).ap()


def make_ident(k, name="ident"):
    idf = k.sb(name + "_f", [128, 128], F32)
    ident = k.sb(name, [128, 128], BF16)
    k.op("pool", lambda e: e.memset(idf[:], 1.0), writes=[idf])
    k.op("pool", lambda e: e.affine_select(out=idf[:], in_=idf[:], pattern=[[-1, 128]],
                                            compare_op=ALU.is_equal, fill=0.0, base=0,
                                            channel_multiplier=1), reads=[idf], writes=[idf])
    k.op("dve", lambda e: e.tensor_copy(ident[:], idf[:]), reads=[idf], writes=[ident])
    return ident, idf


def emit_norm_T(k, xs, hT_dst, hT_t, A, B, ident, scr, ss, rs, xn, pst, tmp):
    k.op("act", lambda e: e.activation(scr[:], xs[:], AF.Square, accum_out=ss[:, 0:1]),
         reads=[xs], writes=[scr, ss])
    k.op("act", lambda e: e.activation(rs[:], ss[:], AF.Sqrt, scale=1.0 / D, bias=EPSB[0][:, 0:1]),
         reads=[ss, EPSB[0]], writes=[rs])
    k.op("dve", lambda e: e.reciprocal(rs[:], rs[:]), reads=[rs], writes=[rs])
    k.op("dve", lambda e: e.tensor_scalar(xn[:], xs[:], rs[:, 0:1], None, op0=ALU.mult),
         reads=[xs, rs], writes=[xn])
    for c in range(8):
        k.op("pe", lambda e: e.transpose(pst[:, c * 128:(c + 1) * 128], xn[:, c * 128:(c + 1) * 128], ident[:]),
             reads=[xn, ident], writes=[pst])
    k.op("dve", lambda e: e.tensor_tensor(tmp[:].rearrange("p (c t) -> p c t", c=8),
                                          pst[:].rearrange("p (c t) -> p c t", c=8),
                                          A[:].unsqueeze(2).broadcast_to([128, 8, 128]), ALU.mult),
         reads=[pst, A], writes=[tmp])
    k.op("dve", lambda e: e.tensor_tensor(hT_dst, tmp[:].rearrange("p (c t) -> p c t", c=8),
                                          B[:].unsqueeze(2).broadcast_to([128, 8, 128]), ALU.add),
         reads=[tmp, B], writes=[hT_t])


EPSB = [None]


def make_eps(k):
    t = k.sb("epsb", [128, 1], F32)
    k.op("dve", lambda e: e.memset(t[:], EPS), writes=[t])
    EPSB[0] = t


def load_AB(k, gcol, sccol, shcol, pfx):
    g = k.sb(pfx + "g", [128, 8], F32)
    sc = k.sb(pfx + "sc", [128, 8], F32)
    B = k.sb(pfx + "B", [128, 8], F32)
    A = k.sb(pfx + "A", [128, 8], F32)
    k.dma("sp", g[:], gcol, writes=[g])
    k.dma("sp", sc[:], sccol, writes=[sc])
    k.dma("sp", B[:], shcol, writes=[B])
    k.op("dve", lambda e: e.scalar_tensor_tensor(A[:], sc[:], 1.0, g[:], op0=ALU.add, op1=ALU.mult),
         reads=[sc, g], writes=[A])
    return A, B


def build_ada():
    nc = bass.Bass("TRN2", target_bir_lowering=False)
    ccol = _dram(nc, "ccol", [128, 8])
    adaw = _dram(nc, "adaw", [2, 1024, 6144])
    adab = _dram(nc, "adab", [1, 2 * 6144])
    mod = _dram(nc, "mod", [1, 2 * 6144], kind="ExternalOutput")
    with contextlib.ExitStack() as st:
        k = KB(nc, st)
        cs = k.sb("cs", [128, 8], F32)
        cond = k.sb("cond", [128, 8], F32)
        brow = k.sb("brow", [1, 2 * 6144], F32)
        orow = k.sb("orow", [1, 2 * 6144], F32)
        k.dma("sp", cs[:], ccol, writes=[cs])
        k.dma("sp", brow[:], adab, writes=[brow])
        k.op("act", lambda e: e.activation(cond[:], cs[:], AF.Silu), reads=[cs], writes=[cond])
        wts = [k.sb("wt%d" % i, [128, 8, 512], F32) for i in range(3)]
        pss = [k.ps("ps%d" % i, [1, 512], F32) for i in range(2)]
        n = 0
        for l in range(2):
            for cc in range(12):
                wt = wts[n % 3]
                ps = pss[n % 2]
                q = "sp" if n % 2 == 0 else "act"
                k.dma(q, wt[:], adaw[l].rearrange("(c p) n -> p c n", p=128)[:, :, cc * 512:(cc + 1) * 512],
                      writes=[wt])
                for kc in range(8):
                    k.op("pe", lambda e: e.matmul(ps[:], lhsT=cond[:, kc:kc + 1], rhs=wt[:, kc, :],
                                                  start=(kc == 0), stop=(kc == 7)),
                         reads=[cond, wt], writes=[ps])
                o0 = l * 6144 + cc * 512
                k.op("dve", lambda e: e.tensor_tensor(orow[:, o0:o0 + 512], ps[:], brow[:, o0:o0 + 512], ALU.add),
                     reads=[ps, brow], writes=[orow])
                n += 1
        tok = k.dma("sp", mod, orow[:], reads=[orow])
        k._wait("sp", tok)
    return nc


def build_ffn():
    TT = 256
    NT = 2048 // TT
    NJ = TT // 128
    nc = bass.Bass("TRN2", target_bir_lowering=False)
    xin = _dram(nc, "xin", [2048, 1024])
    gcol = _dram(nc, "gcol", [128, 8])
    sccol = _dram(nc, "sccol", [128, 8])
    shcol = _dram(nc, "shcol", [128, 8])
    g2b = _dram(nc, "g2b", [128, 1024])
    wg = _dram(nc, "wg", [1024, FH])
    wu = _dram(nc, "wu", [1024, FH])
    wd = _dram(nc, "wd", [FH, 1024])
    xout = _dram(nc, "xout", [2048, 1024], kind="ExternalOutput")
    with contextlib.ExitStack() as st:
        k = KB(nc, st)
        ident, _ = make_ident(k)
        make_eps(k)
        A, B = load_AB(k, gcol, sccol, shcol, "f")
        g2 = k.sb("g2", [128, 1024], F32)
        k.dma("sp", g2[:], g2b, writes=[g2])
        wg_t = [k.sb("wg%d" % i, [128, FH], BF16) for i in range(8)]
        wu_t = [k.sb("wu%d" % i, [128, FH], BF16) for i in range(8)]
        wd_t = [k.sb("wd%d" % i, [128, 1024], BF16) for i in range(NHC)]
        for i in range(8):
            k.dma("pool", wg_t[i][:], wg[i * 128:(i + 1) * 128, :], writes=[wg_t[i]])
            k.dma("pool", wu_t[i][:], wu[i * 128:(i + 1) * 128, :], writes=[wu_t[i]])
        for i in range(NHC):
            k.dma("pool", wd_t[i][:], wd[i * 128:(i + 1) * 128, :], writes=[wd_t[i]])
        xs = [k.sb("xs%d" % i, [128, 1024], F32) for i in range(2 * NJ)]
        scr = k.sb("scr", [128, 1024], F32)
        tmp = k.sb("tmp", [128, 1024], F32)
        ss = k.sb("ss", [128, 1], F32)
        rs = k.sb("rs", [128, 1], F32)
        xn = k.sb("xn", [128, 1024], BF16)
        hT = k.sb("hT", [128, 8, TT], BF16)
        actT = k.sb("actT", [128, NHC, TT], BF16)
        sg = [k.sb("sg%d" % i, [128, TT], F32) for i in range(2)]
        tmp2 = k.sb("tmp2", [128, 512], F32)
        pst = k.ps("pst", [128, 1024], BF16)
        psg = [k.ps("psg%d" % i, [128, TT], F32) for i in range(2)]
        psu = [k.ps("psu%d" % i, [128, TT], F32) for i in range(2)]
        psd = [k.ps("psd%d" % i, [128, 512], F32) for i in range(2)]
        last = []
        for tt in range(NT):
            for j in range(NJ):
                blk = tt * NJ + j
                x_t = xs[(tt % 2) * NJ + j]
                k.dma("sp", x_t[:], xin[blk * 128:(blk + 1) * 128, :], writes=[x_t])
                emit_norm_T(k, x_t, hT[:, :, j * 128:(j + 1) * 128], hT, A, B, ident, scr, ss, rs, xn, pst, tmp)
            for c in range(NHC):
                pg, pu = psg[c % 2], psu[c % 2]
                for kc in range(8):
                    k.op("pe", lambda e: e.matmul(pg[:], lhsT=wg_t[kc][:, c * 128:(c + 1) * 128], rhs=hT[:, kc, :],
                                                  start=(kc == 0), stop=(kc == 7)),
                         reads=[wg_t[kc], hT], writes=[pg])
                for kc in range(8):
                    k.op("pe", lambda e: e.matmul(pu[:], lhsT=wu_t[kc][:, c * 128:(c + 1) * 128], rhs=hT[:, kc, :],
                                                  start=(kc == 0), stop=(kc == 7)),
                         reads=[wu_t[kc], hT], writes=[pu])
                s_t = sg[c % 2]
                k.op("act", lambda e: e.activation(s_t[:], pg[:], AF.Silu), reads=[pg], writes=[s_t])
                k.op("dve", lambda e: e.tensor_tensor(actT[:, c, :], s_t[:], pu[:], ALU.mult),
                     reads=[s_t, pu], writes=[actT])
            for j in range(NJ):
                blk = tt * NJ + j
                x_t = xs[(tt % 2) * NJ + j]
                for half in range(2):
                    pd = psd[half]
                    for c in range(NHC):
                        k.op("pe", lambda e: e.matmul(pd[:], lhsT=actT[:, c, j * 128:(j + 1) * 128],
                                                      rhs=wd_t[c][:, half * 512:(half + 1) * 512],
                                                      start=(c == 0), stop=(c == NHC - 1)),
                             reads=[actT, wd_t[c]], writes=[pd])
                    k.op("dve", lambda e: e.tensor_tensor(tmp2[:], pd[:], g2[:, half * 512:(half + 1) * 512], ALU.mult),
                         reads=[pd, g2], writes=[tmp2])
                    k.op("pool", lambda e: e.tensor_tensor(x_t[:, half * 512:(half + 1) * 512], tmp2[:],
                                                           x_t[:, half * 512:(half + 1) * 512], ALU.add),
                         reads=[tmp2, x_t], writes=[x_t])
                tok = k.dma("sp", xout[blk * 128:(blk + 1) * 128, :], x_t[:], reads=[x_t])
                last.append(tok)
        for tok in last:
            k._wait("sp", tok)
        print("ffn instr", k.cnt, "waits", k.nwait)
    return nc


def build_post(layer):
    TT = 256
    NT = 2048 // TT
    NJ = TT // 128
    nc = bass.Bass("TRN2", target_bir_lowering=False)
    xin = _dram(nc, "xin", [2048, 1024])
    g1b = _dram(nc, "g1b", [128, 1024])
    wo = _dram(nc, "wo", [1024, 1024])
    if layer == 0:
        ya_d = _dram(nc, "ya", [NOWN, 128, 512])
        yb_d = _dram(nc, "yb", [NOWN, 64, 1024])
    else:
        yc_d = _dram(nc, "yc", [NOWN, 128, 1024])
    gcol = _dram(nc, "gcol", [128, 8])
    sccol = _dram(nc, "sccol", [128, 8])
    shcol = _dram(nc, "shcol", [128, 8])
    g2b = _dram(nc, "g2b", [128, 1024])
    wg = _dram(nc, "wg", [1024, FH])
    wu = _dram(nc, "wu", [1024, FH])
    wd = _dram(nc, "wd", [FH, 1024])
    xout = _dram(nc, "xout", [2048, 1024], kind="ExternalOutput")
    with contextlib.ExitStack() as st:
        k = KB(nc, st)
        ident, _ = make_ident(k)
        make_eps(k)
        A, B = load_AB(k, gcol, sccol, shcol, "f")
        g2 = k.sb("g2", [128, 1024], F32)
        g1 = k.sb("g1", [128, 1024], F32)
        k.dma("sp", g1[:], g1b, writes=[g1])
        if layer == 0:
            woA = [k.sb("woA%d" % i, [128, 1024], BF16) for i in range(4)]
            woB = [k.sb("woB%d" % i, [64, 1024], BF16) for i in range(8)]
            for i in range(4):
                k.dma("pool", woA[i][:], wo[i * 128:(i + 1) * 128, :], writes=[woA[i]])
            for i in range(8):
                k.dma("pool", woB[i][:], wo[512 + i * 64:512 + (i + 1) * 64, :], writes=[woB[i]])
            yA = k.sb("yA", [128, 512], BF16)
            yB = k.sb("yB", [64, 1024], BF16)
        else:
            woC = [k.sb("woC%d" % i, [128, 1024], BF16) for i in range(8)]
            for i in range(8):
                k.dma("pool", woC[i][:], wo[i * 128:(i + 1) * 128, :], writes=[woC[i]])
            yC = k.sb("yC", [128, 1024], BF16)
        k.dma("sp", g2[:], g2b, writes=[g2])
        wg_t = [k.sb("wg%d" % i, [128, FH], BF16) for i in range(8)]
        wu_t = [k.sb("wu%d" % i, [128, FH], BF16) for i in range(8)]
        wd_t = [k.sb("wd%d" % i, [128, 1024], BF16) for i in range(NHC)]
        for i in range(8):
            k.dma("pool", wg_t[i][:], wg[i * 128:(i + 1) * 128, :], writes=[wg_t[i]])
            k.dma("pool", wu_t[i][:], wu[i * 128:(i + 1) * 128, :], writes=[wu_t[i]])
        for i in range(NHC):
            k.dma("pool", wd_t[i][:], wd[i * 128:(i + 1) * 128, :], writes=[wd_t[i]])
        xs = [k.sb("xs%d" % i, [128, 1024], F32) for i in range(NJ)]
        tmp = k.sb("tmp", [128, 1024], F32)
        ss = k.sb("ss", [128, 1], F32)
        rs = k.sb("rs", [128, 1], F32)
        xn = k.sb("xn", [128, 1024], BF16)
        hT = k.sb("hT", [128, 8, TT], BF16)
        actT = k.sb("actT", [128, NHC, TT], BF16)
        sg = [k.sb("sg%d" % i, [128, TT], F32) for i in range(2)]
        tmp2 = k.sb("tmp2", [128, 512], F32)
        pst = k.ps("pst", [128, 1024], BF16)
        psg = [k.ps("psg%d" % i, [128, TT], F32) for i in range(2)]
        psu = [k.ps("psu%d" % i, [128, TT], F32) for i in range(2)]
        psd = [k.ps("psd%d" % i, [128, 512], F32) for i in range(2)]
        last = []
        for tt in range(NT):
            for j in range(NJ):
                blk = tt * NJ + j
                x_t = xs[j]
                k.dma("sp", x_t[:], xin[blk * 128:(blk + 1) * 128, :], writes=[x_t])
                if layer == 0:
                    k.dma("pool", yA[:], ya_d[blk], writes=[yA])
                    k.dma("pool", yB[:], yb_d[blk], writes=[yB])
                    terms = [(yA[:, h * 128:(h + 1) * 128], woA[h], 128, yA) for h in range(4)] + \
                            [(yB[0:64, h * 128:(h + 1) * 128], woB[h], 64, yB) for h in range(8)]
                else:
                    k.dma("pool", yC[:], yc_d[blk], writes=[yC])
                    terms = [(yC[:, h * 128:(h + 1) * 128], woC[h], 128, yC) for h in range(8)]
                for half in range(2):
                    pd = psd[half]
                    for ti, (lh, wt_, kk, yt_) in enumerate(terms):
                        k.op("pe", lambda e: e.matmul(pd[:], lhsT=lh, rhs=wt_[0:kk, half * 512:(half + 1) * 512],
                                                      start=(ti == 0), stop=(ti == len(terms) - 1)),
                             reads=[yt_, wt_], writes=[pd])
                    k.op("dve", lambda e: e.tensor_tensor(tmp2[:], pd[:], g1[:, half * 512:(half + 1) * 512], ALU.mult),
                         reads=[pd, g1], writes=[tmp2])
                    k.op("pool", lambda e: e.tensor_tensor(x_t[:, half * 512:(half + 1) * 512], tmp2[:],
                                                           x_t[:, half * 512:(half + 1) * 512], ALU.add),
                         reads=[tmp2, x_t], writes=[x_t])
                emit_norm_T(k, x_t, hT[:, :, j * 128:(j + 1) * 128], hT, A, B, ident, xn, ss, rs, xn, pst, tmp)
            for c in range(NHC):
                pg, pu = psg[c % 2], psu[c % 2]
                for kc in range(8):
                    k.op("pe", lambda e: e.matmul(pg[:], lhsT=wg_t[kc][:, c * 128:(c + 1) * 128], rhs=hT[:, kc, :],
                                                  start=(kc == 0), stop=(kc == 7)),
                         reads=[wg_t[kc], hT], writes=[pg])
                for kc in range(8):
                    k.op("pe", lambda e: e.matmul(pu[:], lhsT=wu_t[kc][:, c * 128:(c + 1) * 128], rhs=hT[:, kc, :],
                                                  start=(kc == 0), stop=(kc == 7)),
                         reads=[wu_t[kc], hT], writes=[pu])
                s_t = sg[c % 2]
                k.op("act", lambda e: e.activation(s_t[:], pg[:], AF.Silu), reads=[pg], writes=[s_t])
                k.op("dve", lambda e: e.tensor_tensor(actT[:, c, :], s_t[:], pu[:], ALU.mult),
                     reads=[s_t, pu], writes=[actT])
            for j in range(NJ):
                blk = tt * NJ + j
                x_t = xs[j]
                for half in range(2):
                    pd = psd[half]
                    for c in range(NHC):
                        k.op("pe", lambda e: e.matmul(pd[:], lhsT=actT[:, c, j * 128:(j + 1) * 128],
                                                      rhs=wd_t[c][:, half * 512:(half + 1) * 512],
                                                      start=(c == 0), stop=(c == NHC - 1)),
                             reads=[actT, wd_t[c]], writes=[pd])
                    k.op("dve", lambda e: e.tensor_tensor(tmp2[:], pd[:], g2[:, half * 512:(half + 1) * 512], ALU.mult),
                         reads=[pd, g2], writes=[tmp2])
                    k.op("pool", lambda e: e.tensor_tensor(x_t[:, half * 512:(half + 1) * 512], tmp2[:],
                                                           x_t[:, half * 512:(half + 1) * 512], ALU.add),
                         reads=[tmp2, x_t], writes=[x_t])
                tok = k.dma("sp", xout[blk * 128:(blk + 1) * 128, :], x_t[:], reads=[x_t])
                last.append(tok)
        for tok in last:
            k._wait("sp", tok)
        print("post instr", k.cnt, "waits", k.nwait)
    return nc


BIG = 30000.0
A_SLOPES = [2.0 ** (-2.0 * (h + 1)) for h in range(4)]
B_SLOPES = [2.0 ** (-(h + 1)) for h in range(8)]
C_SLOPES = [2.0 ** (-0.5 * (h + 1)) for h in range(16)]


def emit_rstd(k, ssum, n_inv, dst):
    np_ = ssum.t.shape[0] if hasattr(ssum.t, "shape") else 128
    k.op("act", lambda e: e.activation(dst[:], ssum[:], AF.Sqrt, scale=n_inv, bias=EPSB[0][0:np_, 0:1]),
         reads=[ssum, EPSB[0]], writes=[dst])
    k.op("dve", lambda e: e.reciprocal(dst[:], dst[:]), reads=[dst], writes=[dst])


def build_mix0(stage=9, nslots=NOWN):
    nc = bass.Bass("TRN2", target_bir_lowering=False)
    x_all = _dram(nc, "x_all", [4096, 1024])
    x_own = _dram(nc, "x_own", [2048, 1024])
    gcol = _dram(nc, "gcol", [128, 8])
    sccol = _dram(nc, "sccol", [128, 8])
    shcol = _dram(nc, "shcol", [128, 8])
    w_in = _dram(nc, "w_in", [1024, 2760])
    kn2_d = _dram(nc, "kn2", [128, 1])
    qn2_d = _dram(nc, "qn2", [128, 1])
    knb_d = _dram(nc, "knb", [64, 1])
    qnb_d = _dram(nc, "qnb", [64, 1])
    subln_d = _dram(nc, "subln", [128, 1])
    lam_d = _dram(nc, "lamv", [128, 4 * 64])
    maskD_d = _dram(nc, "maskD", [128, 2 * 128])
    biasA_d = _dram(nc, "biasA", [128, 4 * 32])
    kaug_d = _dram(nc, "kaug", [4, 4096])
    qaug_d = _dram(nc, "qaug", [2, 1024])
    ns_d = _dram(nc, "nsI", [128, 1024])
    kb1_d = _dram(nc, "kb1", [128, 32])
    ya_o = _dram(nc, "ya", [NOWN, 128, 512], kind="ExternalOutput")
    yb_o = _dram(nc, "yb", [NOWN, 64, 1024], kind="ExternalOutput")
    with contextlib.ExitStack() as st:
        k = KB(nc, st)
        ident, idf = make_ident(k)
        make_eps(k)
        A, B = load_AB(k, gcol, sccol, shcol, "m")
        def ld(name, shape, src, dt=F32, q="sp"):
            t = k.sb(name, shape, dt)
            k.dma(q if dt == F32 else "pool", t[:], src, writes=[t])
            return t
        kn2 = ld("kn2", [128, 1], kn2_d)
        qn2 = ld("qn2", [128, 1], qn2_d)
        knb = ld("knb", [64, 1], knb_d)
        qnb = ld("qnb", [64, 1], qnb_d)
        subln = ld("subln", [128, 1], subln_d)
        lamv = ld("lamv", [128, 256], lam_d)
        maskDf = ld("maskDf", [128, 256], maskD_d)
        maskDb = ld("maskDb", [128, 256], maskD_d, BF16)
        biasA = ld("biasA", [128, 128], biasA_d)
        nsI = ld("nsI", [128, 1024], ns_d, BF16)
        kb1 = ld("kb1", [128, 32], kb1_d)
        I4 = k.sb("I4", [128, 512], BF16)
        k.op("dve", lambda e: e.tensor_copy(I4[:].rearrange("p (a q) -> p a q", a=4),
                                            ident[:].unsqueeze(1).broadcast_to([128, 4, 128])),
             reads=[ident], writes=[I4])
        ones_b = k.sb("ones_b", [128, 128], BF16)
        ones_f = k.sb("ones_f", [128, 128], F32)
        k.op("pool", lambda e: e.memset(ones_b[:], 1.0), writes=[ones_b])
        zeros_b = k.sb("zeros_b", [128, 128], BF16)
        k.op("pool", lambda e: e.memset(zeros_b[:], 0.0), writes=[zeros_b])
        k.op("pool", lambda e: e.memset(ones_f[:], 1.0), writes=[ones_f])
        Esel = k.sb("Esel", [65, 64], F32)
        k.op("pool", lambda e: e.memset(Esel[:], 0.0), writes=[Esel])
        k.op("pool", lambda e: e.memset(Esel[64:65, :], 1.0), reads=[Esel], writes=[Esel])
        lt = k.sb("lt", [128, 128], F32)
        ls = k.sb("ls", [128, 2], F32)
        neglam = k.sb("neglam", [128, 1], F32)
        k.op("dve", lambda e: e.tensor_tensor(lt[:].rearrange("p (a d) -> p a d", a=2),
                                              lamv[:].rearrange("p (a b d) -> p a b d", a=2, b=2)[:, :, 0, :],
                                              lamv[:].rearrange("p (a b d) -> p a b d", a=2, b=2)[:, :, 1, :], ALU.mult),
             reads=[lamv], writes=[lt])
        k.op("dve", lambda e: e.tensor_reduce(ls[:], lt[:].rearrange("p (a d) -> p a d", a=2), axis=AX.X, op=ALU.add),
             reads=[lt], writes=[ls])
        k.op("act", lambda e: e.activation(ls[:], ls[:], AF.Exp), reads=[ls], writes=[ls])
        k.op("dve", lambda e: e.scalar_tensor_tensor(neglam[:], ls[:, 1:2], -0.2, ls[:, 0:1], op0=ALU.add, op1=ALU.subtract),
             reads=[ls], writes=[neglam])
        sgain = k.sb("sgain", [128, 1], F32)
        k.op("dve", lambda e: e.tensor_scalar(sgain[:], subln[:], 0.8, None, op0=ALU.mult), reads=[subln], writes=[sgain])

        WQ = 1544
        w_t = [k.sb("w%d" % i, [128, WQ], BF16) for i in range(8)]
        akT = k.sb("akT", [128, 4, 4096], BF16)
        av = k.sb("av", [128, NBLK, 512], BF16)
        bkT = k.sb("bkT", [68, 4096], BF16)
        ikT = k.sb("ikT", [64, 4096], BF16)
        bv = k.sb("bv", [128, NBLK, 65], BF16)
        k.op("pool", lambda e: e.memset(bv[:], 1.0), writes=[bv])
        k.dma("pool", bkT[64:68, :], kaug_d, reads=[], writes=[bkT])
        akT_b = [T(akT.t[:, :, kb * 128:(kb + 1) * 128]) for kb in range(NBLK)]
        av_b = [T(av.t[:, kb, :]) for kb in range(NBLK)]
        bkT_b = [T(bkT.t[:, kb * 128:(kb + 1) * 128]) for kb in range(NBLK)]
        for t_ in bkT_b:
            t_.w = list(bkT.w)
        ikT_b = [T(ikT.t[:, kb * 128:(kb + 1) * 128]) for kb in range(NBLK)]
        bv_b = [T(bv.t[:, kb, :]) for kb in range(NBLK)]
        for t_ in bv_b:
            t_.w = list(bv.w)
        for i in range(8):
            r = slice(i * 128, (i + 1) * 128)
            k.dma("pool", w_t[i][:, 0:1024], w_in[r, 512:1536], writes=[w_t[i]])
            tk = k.dma("pool", w_t[i][:, 1024:1152], w_in[r, 2048:2176], writes=[])
            w_t[i].w.append(tk)
            tk = k.dma("pool", w_t[i][:, 1152:1216], w_in[r, 2688:2752], writes=[])
            w_t[i].w.append(tk)
        xs = [k.sb("xs%d" % i, [128, 1024], F32) for i in range(2)]
        scr = k.sb("scr", [128, 1024], F32)
        tmp = k.sb("tmp", [128, 1024], F32)
        ss = k.sb("ss", [128, 1], F32)
        rs = k.sb("rs", [128, 1], F32)
        xn = k.sb("xn", [128, 1024], BF16)
        hT = [k.sb("hT%d" % i, [128, 8, 128], BF16) for i in range(2)]
        pf32 = k.sb("pf32", [128, 512], F32)
        sq = k.sb("sq", [128, 512], F32)
        g8 = k.sb("g8", [128, 8], F32)
        r8 = k.sb("r8", [128, 8], F32)
        nb16 = k.sb("nb16", [128, 512], BF16)
        bf32 = k.sb("bf32", [128, 192], F32)
        g1 = k.sb("g1", [128, 1], F32)
        r1 = k.sb("r1", [128, 1], F32)
        bn16 = k.sb("bn16", [128, 128], BF16)
        BB = k.ps("BB", [128, 2, 512], F32)
        Bk = [T(BB.t[:, 0, :]), T(BB.t[:, 1, :])] + [k.ps("B%d" % i, [128, 512], F32) for i in range(2, 7)]
        BT = k.ps("BT", [128, 1024], BF16)

        def qknorm(ps, ngrp, dst16):
            n = ngrp * 64
            k.op("act", lambda e: e.copy(pf32[:, 0:n], ps[:, 0:n]), reads=[ps], writes=[pf32])
            k.op("dve", lambda e: e.tensor_tensor(sq[:, 0:n], pf32[:, 0:n], pf32[:, 0:n], ALU.mult), reads=[pf32], writes=[sq])
            k.op("dve", lambda e: e.tensor_reduce(g8[:, 0:ngrp], sq[:, 0:n].rearrange("p (g d) -> p g d", g=ngrp),
                                                  axis=AX.X, op=ALU.add), reads=[sq], writes=[g8])
            k.op("act", lambda e: e.activation(r8[:, 0:ngrp], g8[:, 0:ngrp], AF.Sqrt, scale=1.0 / 64, bias=EPSB[0][:, 0:1]),
                 reads=[g8, EPSB[0]], writes=[r8])
            k.op("dve", lambda e: e.reciprocal(r8[:, 0:ngrp], r8[:, 0:ngrp]), reads=[r8], writes=[r8])
            k.op("dve", lambda e: e.tensor_tensor(dst16[0].rearrange("p (g d) -> p g d", g=ngrp),
                                                  pf32[:, 0:n].rearrange("p (g d) -> p g d", g=ngrp),
                                                  r8[:, 0:ngrp].unsqueeze(2).broadcast_to([128, ngrp, 64]), ALU.mult),
                 reads=[pf32, r8], writes=[dst16[1]])

        for kb in range(2 * nslots):
            x_t = xs[kb % 2]
            h_t = hT[kb % 2]
            k.dma("sp", x_t[:], x_all[kb * 128:(kb + 1) * 128, :], writes=[x_t])
            emit_norm_T(k, x_t, h_t[:, :, :], h_t, A, B, ident, scr, ss, rs, xn, BT, tmp)
            pak, pav, pb = Bk[0], Bk[1], Bk[2]
            for kc in range(8):
                k.op("pe", lambda e: e.matmul(pak[:], lhsT=h_t[:, kc, :], rhs=w_t[kc][:, 0:512], start=(kc == 0), stop=(kc == 7)),
                     reads=[h_t, w_t[kc]], writes=[pak])
            for kc in range(8):
                k.op("pe", lambda e: e.matmul(pav[:], lhsT=h_t[:, kc, :], rhs=w_t[kc][:, 512:1024], start=(kc == 0), stop=(kc == 7)),
                     reads=[h_t, w_t[kc]], writes=[pav])
            for kc in range(8):
                k.op("pe", lambda e: e.matmul(pb[:, 0:192], lhsT=h_t[:, kc, :], rhs=w_t[kc][:, 1024:1216], start=(kc == 0), stop=(kc == 7)),
                     reads=[h_t, w_t[kc]], writes=[pb])
            k.op("act", lambda e: e.copy(av_b[kb][:], pav[:]), reads=[pav], writes=[av_b[kb]])
            qknorm(pak, 8, (nb16[:, :], nb16))
            for h in range(4):
                k.op("pe", lambda e: e.transpose(BT[:, h * 128:(h + 1) * 128], nb16[:, h * 128:(h + 1) * 128], ident[:]),
                     reads=[nb16, ident], writes=[BT])
            k.op("act", lambda e: e.activation(akT_b[kb][:], BT[:, 0:512].rearrange("p (h t) -> p h t", h=4), AF.Identity,
                                               scale=kn2[:, 0:1]), reads=[BT, kn2], writes=[akT_b[kb]])
            k.op("act", lambda e: e.copy(bf32[:], pb[:, 0:192]), reads=[pb], writes=[bf32])
            k.op("dve", lambda e: e.tensor_tensor(sq[:, 0:64], bf32[:, 0:64], bf32[:, 0:64], ALU.mult), reads=[bf32], writes=[sq])
            k.op("dve", lambda e: e.tensor_reduce(g1[:], sq[:, 0:64], axis=AX.X, op=ALU.add), reads=[sq], writes=[g1])
            k.op("act", lambda e: e.activation(r1[:], g1[:], AF.Sqrt, scale=1.0 / 64, bias=EPSB[0][:, 0:1]),
                 reads=[g1, EPSB[0]], writes=[r1])
            k.op("dve", lambda e: e.reciprocal(r1[:], r1[:]), reads=[r1], writes=[r1])
            k.op("dve", lambda e: e.tensor_scalar(bn16[:, 0:64], bf32[:, 0:64], r1[:, 0:1], None, op0=ALU.mult),
                 reads=[bf32, r1], writes=[bn16])
            k.op("dve", lambda e: e.tensor_copy(bn16[:, 64:128], bf32[:, 128:192]), reads=[bf32], writes=[bn16])
            k.op("pool", lambda e: e.tensor_copy(bv_b[kb][:, 0:64], bf32[:, 64:128]), reads=[bf32], writes=[bv_b[kb]])
            k.op("pe", lambda e: e.transpose(BT[0:64, 512:640], bn16[:, 0:64], ident[:]), reads=[bn16, ident], writes=[BT])
            k.op("pe", lambda e: e.transpose(BT[0:64, 640:768], bn16[:, 64:128], ident[:]), reads=[bn16, ident], writes=[BT])
            k.op("act", lambda e: e.activation(bkT_b[kb][0:64, :], BT[0:64, 512:640], AF.Identity, scale=knb[:, 0:1]),
                 reads=[BT, knb], writes=[bkT_b[kb]])
            k.op("dve", lambda e: e.tensor_copy(ikT_b[kb][:], BT[0:64, 640:768]), reads=[BT], writes=[ikT_b[kb]])

        for i in range(8):
            r = slice(i * 128, (i + 1) * 128)
            k.dma("pool", w_t[i][:, 0:512], w_in[r, 0:512], writes=[w_t[i]])
            for (a, b, c, d) in ((512, 1024, 1536, 2048), (1024, 1536, 2176, 2688), (1536, 1544, 2752, 2760)):
                tk = k.dma("pool", w_t[i][:, a:b], w_in[r, c:d], writes=[])
                w_t[i].w.append(tk)
        score = k.sb("score", [128, 4096], F32)
        mb = k.sb("mb", [128, 4096], BF16)
        R = [k.sb("R%d" % i, [128, 512], BF16) for i in range(8)]
        PT = k.sb("PT", [128, 1024], BF16)
        aqT = k.sb("aqT", [128, 4, 128], BF16)
        bqT = k.sb("bqT", [68, 1024], BF16)
        k.dma("pool", bqT[66:68, :], qaug_d, writes=[bqT])
        iqT = k.sb("iqT", [64, 1024], BF16)
        diag = k.sb("diag", [128, 8, 128], BF16)
        iws = k.sb("iws", [128, 8], F32)
        absw = k.sb("absw", [128, 8], F32)
        sgn = k.sb("sgn", [128, 8], F32)
        iq16 = k.sb("iq16", [128, 512], BF16)
        vals = k.sb("vals", [128, 66], BF16)
        k.op("pool", lambda e: e.memset(vals[:], 0.0), writes=[vals])
        k.op("pool", lambda e: e.memset(vals[:, 65:66], 127.0 * 8.0), reads=[vals], writes=[vals])
        lo = k.sb("lo", [128, 1], F32)
        mid = k.sb("mid", [128, 1], F32)
        cnt = k.sb("cnt", [128, 1], F32)
        ge = k.sb("ge", [128, 1], F32)
        bm = k.sb("bm", [128, 32], F32)
        bmx = k.sb("bmx", [128, 1], F32)
        yaT = k.sb("yaT", [128, 512], F32)
        ybT = k.sb("ybT", [64, 1024], F32)
        a_t = k.sb("a_t", [128, 512], F32)
        rsl = k.sb("rsl", [128, 512], F32)
        last = []

        for t in range(nslots if stage >= 2 else 0):
            nkb = 2 * t + 2
            nk = nkb * 128
            x_t = xs[t % 2]
            h_t = hT[t % 2]
            k.dma("sp", x_t[:], x_own[t * 128:(t + 1) * 128, :], writes=[x_t])
            emit_norm_T(k, x_t, h_t[:, :, :], h_t, A, B, ident, scr, ss, rs, xn, BT, tmp)
            paq, pbq, piq, piw = Bk[0], Bk[1], Bk[2], Bk[3]
            for (ps, c0, c1) in ((paq, 0, 512), (pbq, 512, 1024), (piq, 1024, 1536)):
                for kc in range(8):
                    k.op("pe", lambda e: e.matmul(ps[:], lhsT=h_t[:, kc, :], rhs=w_t[kc][:, c0:c1], start=(kc == 0), stop=(kc == 7)),
                         reads=[h_t, w_t[kc]], writes=[ps])
            for kc in range(8):
                k.op("pe", lambda e: e.matmul(piw[:, 0:8], lhsT=h_t[:, kc, :], rhs=w_t[kc][:, 1536:1544], start=(kc == 0), stop=(kc == 7)),
                     reads=[h_t, w_t[kc]], writes=[piw])
            qknorm(paq, 8, (nb16[:, :], nb16))
            for h in range(4):
                k.op("pe", lambda e: e.transpose(BT[:, h * 128:(h + 1) * 128], nb16[:, h * 128:(h + 1) * 128], ident[:]),
                     reads=[nb16, ident], writes=[BT])
            k.op("act", lambda e: e.activation(aqT[:], BT[:, 0:512].rearrange("p (h t) -> p h t", h=4), AF.Identity,
                                               scale=qn2[:, 0:1]), reads=[BT, qn2], writes=[aqT])
            qknorm(pbq, 8, (nb16[:, :], nb16))
            for h in range(8):
                k.op("pe", lambda e: e.transpose(BT[0:64, h * 128:(h + 1) * 128], nb16[:, h * 64:(h + 1) * 64], ident[:]),
                     reads=[nb16, ident], writes=[BT])
            k.op("act", lambda e: e.activation(bqT[0:64, :], BT[0:64, :], AF.Identity, scale=qnb[:, 0:1]),
                 reads=[BT, qnb], writes=[bqT])
            k.op("act", lambda e: e.copy(iws[:], piw[:, 0:8]), reads=[piw], writes=[iws])
            k.op("act", lambda e: e.activation(sgn[:], iws[:], AF.Sign), reads=[iws], writes=[sgn])
            k.op("dve", lambda e: e.scalar_tensor_tensor(absw[:], iws[:], 0.125 * 8.0 ** -0.5, sgn[:], op0=ALU.mult, op1=ALU.mult),
                 reads=[iws, sgn], writes=[absw])
            k.op("dve", lambda e: e.tensor_tensor(iq16[:].rearrange("p (g d) -> p g d", g=8),
                                                  piq[:].rearrange("p (g d) -> p g d", g=8),
                                                  absw[:].unsqueeze(2).broadcast_to([128, 8, 64]), ALU.mult),
                 reads=[piq, absw], writes=[iq16])
            for h in range(8):
                k.op("pe", lambda e: e.transpose(BT[0:64, h * 128:(h + 1) * 128], iq16[:, h * 64:(h + 1) * 64], ident[:]),
                     reads=[iq16, ident], writes=[BT])
            k.op("dve", lambda e: e.tensor_copy(iqT[:], BT[0:64, :]), reads=[BT], writes=[iqT])
            k.op("dve", lambda e: e.tensor_tensor(diag[:], ident[:].unsqueeze(1).broadcast_to([128, 8, 128]),
                                                  sgn[:].unsqueeze(2).broadcast_to([128, 8, 128]), ALU.mult),
                 reads=[ident, sgn], writes=[diag])

            if stage < 2.2:
                continue
            pS = (Bk[0], Bk[1])
            pY = (Bk[2], Bk[3])
            pD = (Bk[4], Bk[5])
            for kb in range(nkb):
                dg = kb >= 2 * t
                if dg:
                    dsel = kb - 2 * t
                    for i in range(2):
                        k.op("pe", lambda e: e.matmul(pS[i][:], lhsT=maskDb[:, dsel * 128:(dsel + 1) * 128], rhs=I4[:],
                                                      start=True, stop=False), reads=[maskDb, I4], writes=[pS[i]])
                for h in range(4):
                    for m in range(2):
                        i, off = m, h * 128
                        k.op("pe", lambda e: e.matmul(pS[i][:, off:off + 128], lhsT=akT_b[kb][m * 64:(m + 1) * 64, h, :],
                                                      rhs=aqT[m * 64:(m + 1) * 64, h, :], start=(not dg), stop=(not dg) or h == 3),
                             reads=[akT_b[kb], aqT], writes=[pS[i]])
                di = kb - 2 * t + 30
                for h in range(4):
                    k.op("act", lambda e: e.activation(PT[:, h * 256:(h + 1) * 256].rearrange("p (m q) -> p m q", m=2),
                                                       BB.t[:, :, h * 128:(h + 1) * 128], AF.Exp,
                                                       bias=biasA[:, h * 32 + di:h * 32 + di + 1], scale=0.125),
                         reads=[pS[0], pS[1], biasA], writes=[PT])
                if kb == 0:
                    for i in range(2):
                        k.op("pe", lambda e: e.matmul(pY[i][:], lhsT=zeros_b[:], rhs=PT[:, 0:512], start=True, stop=False),
                             reads=[zeros_b, PT], writes=[pY[i]])
                for h in range(4):
                    i, off = h // 2, (h % 2) * 256
                    k.op("pe", lambda e: e.matmul(pY[i][:, off:off + 256], lhsT=av_b[kb][:, h * 128:(h + 1) * 128],
                                                  rhs=PT[:, h * 256:(h + 1) * 256], start=False, stop=(kb == nkb - 1 and h % 2 == 1)),
                         reads=[av_b[kb], PT], writes=[pY[i]])
                for i in range(2):
                    k.op("pe", lambda e: e.matmul(pD[i][:], lhsT=ones_b[:], rhs=PT[:, i * 512:(i + 1) * 512],
                                                  start=(kb == 0), stop=(kb == nkb - 1)), reads=[ones_b, PT], writes=[pD[i]])
            if stage < 3:
                continue
            for i in range(2):
                k.op("dve", lambda e: e.reciprocal(scr[:, i * 512:(i + 1) * 512], pD[i][:]), reads=[pD[i]], writes=[scr])
                k.op("dve", lambda e: e.tensor_tensor(tmp[:, i * 512:(i + 1) * 512], pY[i][:], scr[:, i * 512:(i + 1) * 512], ALU.mult),
                     reads=[pY[i], scr], writes=[tmp])
            tv = tmp[:].rearrange("p (h m q) -> p h m q", h=4, m=2)
            k.op("dve", lambda e: e.scalar_tensor_tensor(a_t[:].rearrange("p (h q) -> p h q", h=4), tv[:, :, 1, :], neglam[:, 0:1],
                                                         tv[:, :, 0, :], op0=ALU.mult, op1=ALU.add),
                 reads=[tmp, neglam], writes=[a_t])
            k.op("dve", lambda e: e.tensor_tensor(rsl[:], a_t[:], a_t[:], ALU.mult), reads=[a_t], writes=[rsl])
            k.op("pe", lambda e: e.matmul(Bk[6][:], lhsT=ones_f[:], rhs=rsl[:], start=True, stop=True),
                 reads=[ones_f, rsl], writes=[Bk[6]])
            k.op("act", lambda e: e.activation(rsl[:], Bk[6][:], AF.Sqrt, scale=1.0 / 128, bias=EPSB[0][:, 0:1]),
                 reads=[Bk[6], EPSB[0]], writes=[rsl])
            k.op("dve", lambda e: e.reciprocal(rsl[:], rsl[:]), reads=[rsl], writes=[rsl])
            k.op("dve", lambda e: e.tensor_tensor(a_t[:], a_t[:], rsl[:], ALU.mult), reads=[a_t, rsl], writes=[a_t])
            k.op("act", lambda e: e.activation(yaT[:], a_t[:], AF.Identity, scale=sgain[:, 0:1]), reads=[a_t, sgain], writes=[yaT])
            last.append(k.dma("sp", ya_o[t], yaT[:], reads=[yaT]))

            if stage < 4:
                continue
            nch = (nkb + 3) // 4
            for c in range(nch):
                W = min(512, nk - c * 512)
                ikc = [ikT_b[kb_] for kb_ in range(c * 4, c * 4 + W // 128)]
                for h in range(8):
                    pL = Bk[h % 2]
                    k.op("pe", lambda e: e.matmul(pL[:, 0:W], lhsT=iqT[:, h * 128:(h + 1) * 128], rhs=ikT[:, c * 512:c * 512 + W],
                                                  start=True, stop=True), reads=[iqT] + ikc, writes=[pL])
                    if h % 2 == 0:
                        k.op("act", lambda e: e.activation(R[h][:, 0:W], pL[:, 0:W], AF.Relu), reads=[pL], writes=[R[h]])
                    else:
                        k.op("dve", lambda e: e.tensor_scalar(R[h][:, 0:W], pL[:, 0:W], 0.0, None, op0=ALU.max), reads=[pL], writes=[R[h]])
                pSc = Bk[2 + c % 2]
                for h in range(8):
                    k.op("pe", lambda e: e.matmul(pSc[:, 0:W], lhsT=diag[:, h, :], rhs=R[h][:, 0:W], start=(h == 0), stop=(h == 7)),
                         reads=[diag, R[h]], writes=[pSc])
                if c < nch - 1:
                    k.op("act", lambda e: e.copy(score[:, c * 512:c * 512 + W], pSc[:, 0:W]), reads=[pSc], writes=[score])
                else:
                    if W > 256:
                        k.op("act", lambda e: e.copy(score[:, c * 512:c * 512 + W - 256], pSc[:, 0:W - 256]), reads=[pSc], writes=[score])
                    k.op("dve", lambda e: e.tensor_tensor(score[:, nk - 256:nk], pSc[:, W - 256:W], maskDf[:], ALU.add),
                         reads=[pSc, maskDf], writes=[score])
            if stage < 5:
                continue
            k.op("dve", lambda e: e.memset(lo[:], -64.0), writes=[lo])
            for it in range(18):
                ci = 64.0 / (2 ** it)
                k.op("dve", lambda e: e.tensor_scalar(mid[:], lo[:], ci, None, op0=ALU.add), reads=[lo], writes=[mid])
                k.op("dve", lambda e: e.tensor_scalar(mb[:, 0:nk], score[:, 0:nk], mid[:, 0:1], None, op0=ALU.is_ge, op1=ALU.add,
                                                      accum_out=cnt[:, 0:1]), reads=[score, mid], writes=[mb, cnt])
                k.op("dve", lambda e: e.tensor_scalar(ge[:], cnt[:], 256.0, ci, op0=ALU.is_ge, op1=ALU.mult), reads=[cnt], writes=[ge])
                k.op("dve", lambda e: e.tensor_tensor(lo[:], lo[:], ge[:], ALU.add), reads=[lo, ge], writes=[lo])
            k.op("dve", lambda e: e.tensor_scalar(mb[:, 0:nk], score[:, 0:nk], lo[:, 0:1], -BIG, op0=ALU.is_lt, op1=ALU.mult),
                 reads=[score, lo], writes=[mb])
            k.op("dve", lambda e: e.tensor_reduce(bm[:, 0:nkb], mb[:, 0:nk].rearrange("p (b k) -> p b k", b=nkb), axis=AX.X, op=ALU.max),
                 reads=[mb], writes=[bm])
            k.op("dve", lambda e: e.tensor_scalar(bm[:, 0:nkb], bm[:, 0:nkb], -1.0, None, op0=ALU.is_ge), reads=[bm], writes=[bm])
            k.op("dve", lambda e: e.tensor_tensor(bm[:, 0:nkb], bm[:, 0:nkb], kb1[:, 0:nkb], ALU.mult), reads=[bm, kb1], writes=[bm])
            k.op("dve", lambda e: e.tensor_reduce(bmx[:], bm[:, 0:nkb], axis=AX.X, op=ALU.max), reads=[bm], writes=[bmx])
            k.op("dve", lambda e: e.tensor_scalar(vals[:, 64:65], bmx[:], -1.0, 1024.0, op0=ALU.add, op1=ALU.mult), reads=[bmx], writes=[vals])
            for i in range(2):
                k.op("pe", lambda e: e.matmul(Bk[i][0:66, :], lhsT=vals[:, 0:66], rhs=nsI[:, i * 512:(i + 1) * 512], start=True, stop=True),
                     reads=[vals, nsI], writes=[Bk[i]])
                k.op("act", lambda e: e.copy(bqT[64:66, i * 512:(i + 1) * 512], Bk[i][64:66, :]), reads=[Bk[i]], writes=[bqT])
            if stage < 6:
                continue
            pSb = (Bk[0], Bk[1])
            pYb = (Bk[4], Bk[5])
            for kb in range(nkb):
                for i in range(2):
                    k.op("pe", lambda e: e.matmul(pSb[i][:], lhsT=bkT_b[kb][0:68, :], rhs=bqT[0:68, i * 512:(i + 1) * 512],
                                                  start=True, stop=False), reads=[bkT_b[kb], bqT], writes=[pSb[i]])
                    k.op("pe", lambda e: e.matmul(pSb[i][:], lhsT=mb[:, kb * 128:(kb + 1) * 128], rhs=I4[:], start=False, stop=True),
                         reads=[mb, I4], writes=[pSb[i]])
                    k.op("act", lambda e: e.activation(PT[:, i * 512:(i + 1) * 512], pSb[i][:], AF.Exp, scale=0.125),
                         reads=[pSb[i]], writes=[PT])
                for i in range(2):
                    k.op("pe", lambda e: e.matmul(pYb[i][0:65, :], lhsT=bv_b[kb][:, 0:65], rhs=PT[:, i * 512:(i + 1) * 512],
                                                  start=(kb == 0), stop=(kb == nkb - 1)), reads=[bv_b[kb], PT], writes=[pYb[i]])
            for i in range(2):
                k.op("act", lambda e: e.copy(scr[0:65, i * 512:(i + 1) * 512], pYb[i][0:65, :]), reads=[pYb[i]], writes=[scr])
                k.op("pe", lambda e: e.matmul(Bk[6][0:64, :], lhsT=Esel[:], rhs=scr[0:65, i * 512:(i + 1) * 512], start=True, stop=True),
                     reads=[Esel, scr], writes=[Bk[6]])
                k.op("dve", lambda e: e.reciprocal(tmp[0:64, i * 512:(i + 1) * 512], Bk[6][0:64, :]), reads=[Bk[6]], writes=[tmp])
                k.op("dve", lambda e: e.tensor_tensor(ybT[:, i * 512:(i + 1) * 512], scr[0:64, i * 512:(i + 1) * 512],
                                                      tmp[0:64, i * 512:(i + 1) * 512], ALU.mult), reads=[scr, tmp], writes=[ybT])
            last.append(k.dma("sp", yb_o[t], ybT[:], reads=[ybT]))
        for tok in last:
            k._wait("sp", tok)
        print("mix0 instr", k.cnt, "waits", k.nwait)
    return nc


def _col(v):
    return np.ascontiguousarray(np.asarray(v, np.float32).reshape(8, 128).T)


def _own(a, j):
    return np.ascontiguousarray(a.reshape(16, 2, 128, 1024)[:, j].reshape(2048, 1024))


def _bc(v, n=128):
    return np.ascontiguousarray(np.broadcast_to(np.asarray(v, np.float32).reshape(1, -1), (n, v.size)))


def mix0_consts(j):
    f = np.float32
    q = np.arange(128)[:, None]
    kk = np.arange(128)[None, :]
    tri = np.where(kk <= q, 0.0, -BIG).astype(f)
    full = np.full((128, 128), -BIG, f)
    zero = np.zeros((128, 128), f)
    maskD = np.concatenate([tri, full], 1) if j == 0 else np.concatenate([zero, tri], 1)
    biasA = np.zeros((128, 128), f)
    for h in range(4):
        for di in range(32):
            biasA[:, h * 32 + di] = A_SLOPES[h] * (128.0 * (di - 30) + np.arange(128))
    kpos = np.arange(4096)
    kaug = np.stack([np.ones(4096), np.ones(4096), kpos // 128, kpos % 128]).astype(f)
    qaug = np.zeros((2, 1024), f)
    nsI = np.zeros((128, 1024), f)
    for h in range(8):
        qaug[0, h * 128:(h + 1) * 128] = 8.0 * B_SLOPES[h] * 128.0
        qaug[1, h * 128:(h + 1) * 128] = 8.0 * B_SLOPES[h]
        nsI[:, h * 128:(h + 1) * 128] = -B_SLOPES[h] * np.eye(128)
    kb1 = _bc(np.arange(1, 33, dtype=f))
    return {"maskD": np.ascontiguousarray(maskD), "biasA": biasA, "kaug": kaug, "qaug": qaug, "nsI": nsI, "kb1": kb1}


def mix0_inputs(i, x, mod0, p):
    b, j = i // 2, i % 2
    sh1, sc1 = mod0[b, 0:1024], mod0[b, 1024:2048]
    d = {"x_all": np.ascontiguousarray(x[b]), "x_own": _own(x[b], j),
         "gcol": _col(p["norm_mix"][0]), "sccol": _col(sc1), "shcol": _col(sh1),
         "w_in": np.ascontiguousarray(p["w_in_even"][0]),
         "kn2": np.ascontiguousarray(np.tile(p["kn_a"][0], 2).reshape(128, 1)),
         "qn2": np.ascontiguousarray(np.tile(p["qn_a"][0], 2).reshape(128, 1)),
         "knb": np.ascontiguousarray(p["kn_b"][0].reshape(64, 1)),
         "qnb": np.ascontiguousarray(p["qn_b"][0].reshape(64, 1)),
         "subln": np.ascontiguousarray(p["subln_a"][0].reshape(128, 1)),
         "lamv": _bc(np.concatenate([p["lam_q1"][0], p["lam_k1"][0], p["lam_q2"][0], p["lam_k2"][0]]))}
    d.update(mix0_consts(j))
    return d


def build_mix1():
    nc = bass.Bass("TRN2", target_bir_lowering=False)
    x_all = _dram(nc, "x_all", [4096, 1024])
    x_own = _dram(nc, "x_own", [2048, 1024])
    gcol = _dram(nc, "gcol", [128, 8])
    sccol = _dram(nc, "sccol", [128, 8])
    shcol = _dram(nc, "shcol", [128, 8])
    w_in = _dram(nc, "w_in", [1024, 1536])
    knc_d = _dram(nc, "knc", [64, 1])
    qnc_d = _dram(nc, "qnc", [64, 1])
    maskC_d = _dram(nc, "maskC", [128, 384])
    kaugC_d = _dram(nc, "kaugC", [4, 384])
    qaugC_d = _dram(nc, "qaugC", [4, 2048])
    sinkb_d = _dram(nc, "sinkb", [128, 2048])
    yc_o = _dram(nc, "yc", [NOWN, 128, 1024], kind="ExternalOutput")
    with contextlib.ExitStack() as st:
        k = KB(nc, st)
        ident, idf = make_ident(k)
        make_eps(k)
        A, B = load_AB(k, gcol, sccol, shcol, "m")

        def ld(name, shape, src, dt=F32):
            t = k.sb(name, shape, dt)
            k.dma("sp" if dt == F32 else "pool", t[:], src, writes=[t])
            return t
        knc = ld("knc", [64, 1], knc_d)
        qnc = ld("qnc", [64, 1], qnc_d)
        maskC = ld("maskC", [128, 384], maskC_d, BF16)
        kaugC = ld("kaugC", [4, 384], kaugC_d, BF16)
        qaugC = ld("qaugC", [4, 2048], qaugC_d, BF16)
        esink = ld("esink", [128, 2048], sinkb_d)
        k.op("act", lambda e: e.activation(esink[:], esink[:], AF.Exp), reads=[esink], writes=[esink])
        I4 = k.sb("I4", [128, 512], BF16)
        k.op("dve", lambda e: e.tensor_copy(I4[:].rearrange("p (a q) -> p a q", a=4),
                                            ident[:].unsqueeze(1).broadcast_to([128, 4, 128])),
             reads=[ident], writes=[I4])
        ones_b = k.sb("ones_b", [128, 128], BF16)
        k.op("pool", lambda e: e.memset(ones_b[:], 1.0), writes=[ones_b])
        w_t = [k.sb("w%d" % i, [128, 1536], BF16) for i in range(8)]
        for i in range(8):
            k.dma("pool", w_t[i][:], w_in[i * 128:(i + 1) * 128, :], writes=[w_t[i]])
        kT = k.sb("kT", [64, 4, 4096], BF16)
        vd = k.sb("vd", [128, NBLK, 512], BF16)
        kT_b = [T(kT.t[:, :, kb * 128:(kb + 1) * 128]) for kb in range(NBLK)]
        vd_b = [T(vd.t[:, kb, :]) for kb in range(NBLK)]
        xs = [k.sb("xs%d" % i, [128, 1024], F32) for i in range(2)]
        scr = k.sb("scr", [128, 1024], F32)
        tmp = k.sb("tmp", [128, 1024], F32)
        ss = k.sb("ss", [128, 1], F32)
        rs = k.sb("rs", [128, 1], F32)
        xn = k.sb("xn", [128, 1024], BF16)
        hT = [k.sb("hT%d" % i, [128, 8, 128], BF16) for i in range(2)]
        pf32 = k.sb("pf32", [128, 1024], F32)
        sq = k.sb("sq", [128, 1024], F32)
        g8 = k.sb("g8", [128, 16], F32)
        r8 = k.sb("r8", [128, 16], F32)
        nb16 = k.sb("nb16", [128, 1024], BF16)
        qT = k.sb("qT", [64, 16, 128], BF16)
        PT = k.sb("PT", [128, 512], BF16)
        rd = k.sb("rd", [128, 512], F32)
        yC = k.sb("yC", [128, 1024], F32)
        Bk = [k.ps("B%d" % i, [128, 512], F32) for i in range(7)]
        BT = k.ps("BT", [128, 1024], BF16)

        def qknorm(pss, ngrp):
            n = ngrp * 64
            o = 0
            for ps, w in pss:
                k.op("act", lambda e: e.copy(pf32[:, o:o + w], ps[:, 0:w]), reads=[ps], writes=[pf32])
                o += w
            k.op("dve", lambda e: e.tensor_tensor(sq[:, 0:n], pf32[:, 0:n], pf32[:, 0:n], ALU.mult), reads=[pf32], writes=[sq])
            k.op("dve", lambda e: e.tensor_reduce(g8[:, 0:ngrp], sq[:, 0:n].rearrange("p (g d) -> p g d", g=ngrp),
                                                  axis=AX.X, op=ALU.add), reads=[sq], writes=[g8])
            k.op("act", lambda e: e.activation(r8[:, 0:ngrp], g8[:, 0:ngrp], AF.Sqrt, scale=1.0 / 64, bias=EPSB[0][:, 0:1]),
                 reads=[g8, EPSB[0]], writes=[r8])
            k.op("dve", lambda e: e.reciprocal(r8[:, 0:ngrp], r8[:, 0:ngrp]), reads=[r8], writes=[r8])
            k.op("dve", lambda e: e.tensor_tensor(nb16[:, 0:n].rearrange("p (g d) -> p g d", g=ngrp),
                                                  pf32[:, 0:n].rearrange("p (g d) -> p g d", g=ngrp),
                                                  r8[:, 0:ngrp].unsqueeze(2).broadcast_to([128, ngrp, 64]), ALU.mult),
                 reads=[pf32, r8], writes=[nb16])

        for kb in range(NBLK):
            x_t = xs[kb % 2]
            h_t = hT[kb % 2]
            k.dma("sp", x_t[:], x_all[kb * 128:(kb + 1) * 128, :], writes=[x_t])
            emit_norm_T(k, x_t, h_t[:, :, :], h_t, A, B, ident, scr, ss, rs, xn, BT, tmp)
            pkv = Bk[0]
            for kc in range(8):
                k.op("pe", lambda e: e.matmul(pkv[:], lhsT=h_t[:, kc, :], rhs=w_t[kc][:, 1024:1536], start=(kc == 0), stop=(kc == 7)),
                     reads=[h_t, w_t[kc]], writes=[pkv])
            for d in range(2):
                k.op("act", lambda e: e.copy(vd_b[kb][:].rearrange("p (v c) -> p v c", v=4)[:, :, d * 64:(d + 1) * 64],
                                             pkv[:, 256:512].rearrange("p (v c) -> p v c", v=4)),
                     reads=[pkv], writes=[vd_b[kb]])
            qknorm([(pkv, 256)], 4)
            for v in range(4):
                k.op("pe", lambda e: e.transpose(BT[0:64, v * 128:(v + 1) * 128], nb16[:, v * 64:(v + 1) * 64], ident[:]),
                     reads=[nb16, ident], writes=[BT])
            k.op("act", lambda e: e.activation(kT_b[kb][:], BT[0:64, 0:512].rearrange("p (v t) -> p v t", v=4), AF.Identity,
                                               scale=knc[:, 0:1]), reads=[BT, knc], writes=[kT_b[kb]])
        last = []
        for t in range(NOWN):
            x_t = xs[t % 2]
            h_t = hT[t % 2]
            k.dma("sp", x_t[:], x_own[t * 128:(t + 1) * 128, :], writes=[x_t])
            emit_norm_T(k, x_t, h_t[:, :, :], h_t, A, B, ident, scr, ss, rs, xn, BT, tmp)
            pq = (Bk[0], Bk[1])
            for i in range(2):
                for kc in range(8):
                    k.op("pe", lambda e: e.matmul(pq[i][:], lhsT=h_t[:, kc, :], rhs=w_t[kc][:, i * 512:(i + 1) * 512],
                                                  start=(kc == 0), stop=(kc == 7)), reads=[h_t, w_t[kc]], writes=[pq[i]])
            qknorm([(pq[0], 512), (pq[1], 512)], 16)
            for r_ in range(2):
                for h in range(8):
                    hh = r_ * 8 + h
                    k.op("pe", lambda e: e.transpose(BT[0:64, h * 128:(h + 1) * 128], nb16[:, hh * 64:(hh + 1) * 64], ident[:]),
                         reads=[nb16, ident], writes=[BT])
                k.op("act", lambda e: e.activation(qT[:, r_ * 8:(r_ + 1) * 8, :], BT[0:64, :].rearrange("p (h t) -> p h t", h=8),
                                                   AF.Identity, scale=qnc[:, 0:1]), reads=[BT, qnc], writes=[qT])
            blks = [(dl, 2 * t + dl) for dl in (-1, 0, 1) if 2 * t + dl >= 0]
            for v in range(4):
                pS, pY, pD = Bk[2 + (v % 2)], Bk[4 + (v % 2)], Bk[6]
                for bi, (dl, kb) in enumerate(blks):
                    k.op("pe", lambda e: e.matmul(pS[:], lhsT=kT_b[kb][:, v, :], rhs=qT[:, v * 4:(v + 1) * 4, :], start=True, stop=False),
                         reads=[kT_b[kb], qT], writes=[pS])
                    k.op("pe", lambda e: e.matmul(pS[:], lhsT=kaugC[:, (dl + 1) * 128:(dl + 2) * 128], rhs=qaugC[:, v * 512:(v + 1) * 512],
                                                  start=False, stop=False), reads=[kaugC, qaugC], writes=[pS])
                    k.op("pe", lambda e: e.matmul(pS[:], lhsT=maskC[:, (dl + 1) * 128:(dl + 2) * 128], rhs=I4[:], start=False, stop=True),
                         reads=[maskC, I4], writes=[pS])
                    k.op("act", lambda e: e.activation(PT[:], pS[:], AF.Exp, scale=0.125), reads=[pS], writes=[PT])
                    k.op("pe", lambda e: e.matmul(pY[:], lhsT=vd_b[kb][:, v * 128:(v + 1) * 128], rhs=PT[:],
                                                  start=(bi == 0), stop=(bi == len(blks) - 1)), reads=[vd_b[kb], PT], writes=[pY])
                    k.op("pe", lambda e: e.matmul(pD[:], lhsT=ones_b[:], rhs=PT[:], start=(bi == 0), stop=(bi == len(blks) - 1)),
                         reads=[ones_b, PT], writes=[pD])
                k.op("dve", lambda e: e.tensor_tensor(rd[:], pD[:], esink[:, v * 512:(v + 1) * 512], ALU.add), reads=[pD, esink], writes=[rd])
                k.op("dve", lambda e: e.reciprocal(rd[:], rd[:]), reads=[rd], writes=[rd])
                for par in range(2):
                    ps_ = slice(par * 64, (par + 1) * 64)
                    k.op("dve", lambda e: e.tensor_tensor(
                        yC[ps_, v * 256:(v + 1) * 256].rearrange("p (c q) -> p c q", c=2),
                        pY[ps_, :].rearrange("p (c r q) -> p c r q", c=2, r=2)[:, :, par, :],
                        rd[ps_, :].rearrange("p (c r q) -> p c r q", c=2, r=2)[:, :, par, :], ALU.mult),
                        reads=[pY, rd], writes=[yC])
            last.append(k.dma("sp", yc_o[t], yC[:], reads=[yC]))
        for tok in last:
            k._wait("sp", tok)
        print("mix1 instr", k.cnt, "waits", k.nwait)
    return nc


def _bf_split(x):
    import ml_dtypes
    hi = x.astype(ml_dtypes.bfloat16).astype(np.float32)
    lo = (x - hi).astype(ml_dtypes.bfloat16).astype(np.float32)
    return hi, lo


def mix1_inputs(i, x, mod1, p):
    f = np.float32
    b, j = i // 2, i % 2
    sh1, sc1 = mod1[b, 0:1024], mod1[b, 1024:2048]
    q = np.arange(128)[:, None]
    kk = np.arange(128)[None, :]
    own = np.where(kk <= q, 0.0, -BIG).astype(f)
    prev = np.where(kk > q, 0.0, -BIG).astype(f)
    full = np.full((128, 128), -BIG, f)
    maskC = np.concatenate([prev, own, full], 1) if j == 0 else np.concatenate([full, prev, own], 1)
    kaugC = np.zeros((4, 384), f)
    for dl in (-1, 0, 1):
        krel = 128.0 * dl + np.arange(128)
        kaugC[0, (dl + 1) * 128:(dl + 2) * 128] = krel
        kaugC[1, (dl + 1) * 128:(dl + 2) * 128] = krel
        kaugC[2:4, (dl + 1) * 128:(dl + 2) * 128] = 1.0
    qaugC = np.zeros((4, 2048), f)
    for h in range(16):
        s = np.asarray([8.0 * C_SLOPES[h]], f)
        shi, slo = _bf_split(s)
        c = (-(shi[0] + slo[0]) * (128.0 * j + np.arange(128))).astype(f)
        chi, clo = _bf_split(c)
        sl = slice(h * 128, (h + 1) * 128)
        qaugC[0, sl] = shi[0]
        qaugC[1, sl] = slo[0]
        qaugC[2, sl] = chi
        qaugC[3, sl] = clo
    sinkb = _bc(np.repeat(np.asarray(p["sinks_c"][0], f), 128))
    return {"x_all": np.ascontiguousarray(x[b]), "x_own": _own(x[b], j),
            "gcol": _col(p["norm_mix"][1]), "sccol": _col(sc1), "shcol": _col(sh1),
            "w_in": np.ascontiguousarray(p["w_in_odd"][0]),
            "knc": np.ascontiguousarray(p["kn_c"][0].reshape(64, 1)),
            "qnc": np.ascontiguousarray(p["qn_c"][0].reshape(64, 1)),
            "maskC": np.ascontiguousarray(maskC), "kaugC": kaugC, "qaugC": qaugC, "sinkb": sinkb}


def _post_inputs(i, l, x, mod, p, ys):
    b, j = i // 2, i % 2
    m = mod[b]
    d = {"xin": _own(x[b], j), "gcol": _col(p["norm_ffn"][l]), "sccol": _col(m[4096:5120]), "shcol": _col(m[3072:4096]),
         "g2b": _bc(m[5120:6144]), "g1b": _bc(m[2048:3072]),
         "wo": np.ascontiguousarray(p["w_out"][l]), "wg": np.ascontiguousarray(p["ffn_gate"][l]),
         "wu": np.ascontiguousarray(p["ffn_up"][l]), "wd": np.ascontiguousarray(p["ffn_down"][l])}
    d.update(ys)
    return d


def _assemble(outs):
    x = np.empty((4, 4096, 1024), np.float32)
    for i in range(8):
        b, j = i // 2, i % 2
        x[b].reshape(16, 2, 128, 1024)[:, j] = outs[i].reshape(16, 128, 1024)
    return x


def kernel(**inputs):
    p = {k_: np.asarray(v, dtype=np.float32) for k_, v in inputs.items()}
    x = np.ascontiguousarray(p["x"])
    cores = list(range(8))
    nc = build_ada()
    maps = [{"ccol": _col(p["c"][i // 2]), "adaw": np.ascontiguousarray(p["ada_w"]),
             "adab": np.ascontiguousarray(p["ada_b"].reshape(1, -1))} for i in cores]
    res = run_bass_kernel_spmd(nc, maps, core_ids=cores)
    mods = [np.stack([res.results[2 * b]["mod"].reshape(2, 6144)[l] for b in range(4)]) for l in range(2)]
    nc = build_mix0()
    res = run_bass_kernel_spmd(nc, [mix0_inputs(i, x, mods[0], p) for i in cores], core_ids=cores)
    ys = [{"ya": res.results[i]["ya"], "yb": res.results[i]["yb"]} for i in cores]
    nc = build_post(0)
    res = run_bass_kernel_spmd(nc, [_post_inputs(i, 0, x, mods[0], p, ys[i]) for i in cores], core_ids=cores)
    x = _assemble([res.results[i]["xout"] for i in cores])
    nc = build_mix1()
    res = run_bass_kernel_spmd(nc, [mix1_inputs(i, x, mods[1], p) for i in cores], core_ids=cores)
    ys = [{"yc": res.results[i]["yc"]} for i in cores]
    nc = build_post(1)
    res = run_bass_kernel_spmd(nc, [_post_inputs(i, 1, x, mods[1], p, ys[i]) for i in cores], core_ids=cores)
    return _assemble([res.results[i]["xout"] for i in cores])
```
